# Optimizing a Trainium2 kernel written in Bass

```python
import jax, jax.numpy as jnp
from jax import lax
import numpy as np

D_MODEL = 4096
BATCH = 1
SEQ = 8192
DEPTH = 1

HEAD_DIM = 128
ATTN_GROUPS = ((128, 1), (512, 4), (2048, 16))
N_ATTN_GROUPS = len(ATTN_GROUPS)
HEADS_PER_GROUP = 4
N_Q_HEADS = N_ATTN_GROUPS * HEADS_PER_GROUP
N_KV_HEADS = HEADS_PER_GROUP
ROPE_THETA = 10000.0
BLOCK = 128
NEG_INF = -1e30
CONV_WIDTH = D_MODEL // 2
CONV_K = 3
PLE_DIM = 256
N_EXPERT_GROUPS = 4
EXPERTS_PER_GROUP = 8
N_EXPERTS = N_EXPERT_GROUPS * EXPERTS_PER_GROUP
TOP_K = 2
D_EXPERT = 1536
EPS = 1e-6

Q_COLS = N_Q_HEADS * HEAD_DIM
KV_COLS = N_KV_HEADS * HEAD_DIM
A_OUT = N_KV_HEADS * HEAD_DIM
IN_COLS = Q_COLS + 2 * KV_COLS + 3 * CONV_WIDTH + 2 * D_MODEL
SPLITS = tuple(int(c) for c in np.cumsum([Q_COLS, KV_COLS, KV_COLS, CONV_WIDTH, CONV_WIDTH, CONV_WIDTH, D_MODEL])[:])

kernel_name = "hybrid_dilated_attn_shortconv_hmoe"


def _rmsnorm(x, g):
    xf = x.astype(jnp.float32)
    y = xf * lax.rsqrt(jnp.mean(xf * xf, axis=-1, keepdims=True) + EPS)
    return (y * g.astype(jnp.float32)).astype(x.dtype)


def _rope(t, pos):
    half = HEAD_DIM // 2
    inv_freq = ROPE_THETA ** (-jnp.arange(half, dtype=jnp.float32) / half)
    ang = pos.astype(jnp.float32)[:, None] * inv_freq[None, :]
    cos = jnp.cos(ang)[None, :, None, :]
    sin = jnp.sin(ang)[None, :, None, :]
    tf = t.astype(jnp.float32)
    t1, t2 = tf[..., :half], tf[..., half:]
    return jnp.concatenate([t1 * cos - t2 * sin, t2 * cos + t1 * sin], axis=-1).astype(t.dtype)


def _dilated_window_attention(q, k, v, window, dilation):
    B, S, H, Dh = q.shape
    L = S // dilation
    W = window // dilation
    nprev = -(-W // BLOCK)
    Lp = -(-L // BLOCK) * BLOCK
    nb = Lp // BLOCK
    N = B * dilation

    def to_sub(t):
        t = t.reshape(B, L, dilation, H, Dh).transpose(0, 2, 1, 3, 4).reshape(N, L, H, Dh)
        return jnp.pad(t, ((0, 0), (0, Lp - L), (0, 0), (0, 0)))

    qs, ks, vs = to_sub(q), to_sub(k), to_sub(v)
    qb = qs.reshape(N, nb, BLOCK, H, Dh)

    def band(t):
        tp = jnp.pad(t, ((0, 0), (nprev * BLOCK, 0), (0, 0), (0, 0))).reshape(N, nb + nprev, BLOCK, H, Dh)
        return jnp.concatenate([tp[:, j:j + nb] for j in range(nprev + 1)], axis=2)

    kb, vb = band(ks), band(vs)
    KB = (nprev + 1) * BLOCK
    scores = jnp.einsum('nbqhd,nbkhd->nbhqk', qb, kb, preferred_element_type=jnp.float32) * (Dh ** -0.5)
    qi = jnp.arange(BLOCK)[:, None]
    kj = jnp.arange(KB)[None, :]
    dist = qi + nprev * BLOCK - kj
    kpos = jnp.arange(nb)[:, None, None] * BLOCK - nprev * BLOCK + kj[None]
    valid = (dist >= 0)[None] & (dist <= W)[None] & (kpos >= 0)
    scores = jnp.where(valid[None, :, None], scores, NEG_INF)
    m = jnp.max(scores, axis=-1, keepdims=True)
    e = jnp.exp(scores - m)
    s = jnp.sum(e, axis=-1, keepdims=True)
    probs = (e / s).astype(v.dtype)
    out = jnp.einsum('nbhqk,nbkhd->nbqhd', probs, vb)
    lse = (m + jnp.log(s))[..., 0].transpose(0, 1, 3, 2)
    out = out.reshape(N, Lp, H, Dh)[:, :L].reshape(B, dilation, L, H, Dh).transpose(0, 2, 1, 3, 4).reshape(B, S, H, Dh)
    lse = lse.reshape(N, Lp, H)[:, :L].reshape(B, dilation, L, H).transpose(0, 2, 1, 3).reshape(B, S, H)
    return out, lse


def _short_conv(u, w_conv):
    S = u.shape[1]
    up = jnp.pad(u, ((0, 0), (CONV_K - 1, 0), (0, 0)))
    return sum(w_conv[j] * up[:, j:j + S] for j in range(CONV_K))


def _hierarchical_moe(h, w_group, b_group, w_router, b_router, w_gate, w_up, w_down):
    B, S, D = h.shape
    T = B * S
    hf = h.reshape(T, D)
    g_logits = (hf @ w_group).astype(jnp.float32) + b_group.astype(jnp.float32)
    g_prob = jax.nn.softmax(g_logits, axis=-1)
    _, g_idx = lax.top_k(g_logits, 1)
    g_w = jnp.take_along_axis(g_prob, g_idx, axis=-1)
    e_all = ((hf @ w_router).astype(jnp.float32) + b_router.astype(jnp.float32)).reshape(T, N_EXPERT_GROUPS, EXPERTS_PER_GROUP)
    e_sel = jnp.take_along_axis(e_all, jnp.broadcast_to(g_idx[:, :, None], (T, 1, EXPERTS_PER_GROUP)), axis=1)[:, 0]
    e_prob = jax.nn.softmax(e_sel, axis=-1)
    top_p, top_i = lax.top_k(e_prob, TOP_K)
    top_p = top_p / jnp.sum(top_p, axis=-1, keepdims=True)
    weights = (g_w * top_p).reshape(-1)
    experts = (g_idx * EXPERTS_PER_GROUP + top_i).reshape(-1).astype(jnp.int32)
    tokens = jnp.repeat(jnp.arange(T, dtype=jnp.int32), TOP_K)
    A = T * TOP_K
    order = jnp.argsort(experts)
    se, st, sw = experts[order], tokens[order], weights[order]
    counts = jax.ops.segment_sum(jnp.ones((A,), jnp.int32), experts, num_segments=N_EXPERTS)
    starts = jnp.cumsum(counts) - counts
    padded = (counts + BLOCK - 1) // BLOCK * BLOCK
    pends = jnp.cumsum(padded)
    pstarts = pends - padded
    dest = pstarts[se] + (jnp.arange(A, dtype=jnp.int32) - starts[se])
    P = A + N_EXPERTS * BLOCK
    nblk = P // BLOCK
    row_tok = jnp.zeros((P,), jnp.int32).at[dest].set(st)
    row_w = jnp.zeros((P,), jnp.float32).at[dest].set(sw)
    blk_exp = jnp.minimum(jnp.searchsorted(pends, jnp.arange(nblk, dtype=jnp.int32) * BLOCK, side='right'), N_EXPERTS - 1)

    def expert_block(args):
        toks, wts, e = args
        xb = hf[toks]
        hid = jax.nn.silu(xb @ w_gate[e]) * (xb @ w_up[e])
        y = hid @ w_down[e]
        return y * wts[:, None].astype(y.dtype)

    yblk = lax.map(expert_block, (row_tok.reshape(nblk, BLOCK), row_w.reshape(nblk, BLOCK), blk_exp))
    out = jnp.zeros((T, D), yblk.dtype).at[row_tok].add(yblk.reshape(P, D))
    return out.reshape(B, S, D).astype(h.dtype)


def setup_inputs(seed: int = 0) -> dict:
    key = jax.random.key(seed)
    ks = jax.random.split(key, 24)
    f32 = jnp.float32

    def nrm(k, shape, fan_in):
        return jax.random.normal(k, shape, f32) * (fan_in ** -0.5)

    def gain(k, shape):
        return 1.0 + 0.02 * jax.random.normal(k, shape, f32)

    return {
        "x": jax.random.normal(ks[0], (BATCH, SEQ, D_MODEL), f32),
        "p": jax.random.normal(ks[1], (DEPTH, BATCH, SEQ, PLE_DIM), f32),
        "w_in": nrm(ks[2], (DEPTH, D_MODEL, IN_COLS), D_MODEL),
        "w_conv": nrm(ks[3], (DEPTH, CONV_K, CONV_WIDTH), CONV_K),
        "w_up_a": nrm(ks[4], (DEPTH, A_OUT, D_MODEL), A_OUT),
        "w_out_b": nrm(ks[5], (DEPTH, CONV_WIDTH, D_MODEL), CONV_WIDTH),
        "w_o": nrm(ks[6], (DEPTH, D_MODEL, D_MODEL), D_MODEL),
        "norm_mix": gain(ks[7], (DEPTH, D_MODEL)),
        "norm_ffn": gain(ks[8], (DEPTH, D_MODEL)),
        "w_group": nrm(ks[9], (DEPTH, D_MODEL, N_EXPERT_GROUPS), D_MODEL),
        "b_group": 0.01 * jax.random.normal(ks[10], (DEPTH, N_EXPERT_GROUPS), f32),
        "w_router": nrm(ks[11], (DEPTH, D_MODEL, N_EXPERTS), D_MODEL),
        "b_router": 0.01 * jax.random.normal(ks[12], (DEPTH, N_EXPERTS), f32),
        "w_gate": nrm(ks[13], (DEPTH, N_EXPERTS, D_MODEL, D_EXPERT), D_MODEL),
        "w_up": nrm(ks[14], (DEPTH, N_EXPERTS, D_MODEL, D_EXPERT), D_MODEL),
        "w_down": nrm(ks[15], (DEPTH, N_EXPERTS, D_EXPERT, D_MODEL), D_EXPERT),
        "norm_ple": gain(ks[16], (DEPTH, D_MODEL)),
        "w_ple_gate": nrm(ks[17], (DEPTH, D_MODEL, D_MODEL), D_MODEL),
        "w_ple_proj": nrm(ks[18], (DEPTH, PLE_DIM, D_MODEL), PLE_DIM),
        "norm_final": gain(ks[19], (D_MODEL,)),
    }


def reference(x, p, w_in, w_conv, w_up_a, w_out_b, w_o, norm_mix, norm_ffn, w_group, b_group,
              w_router, b_router, w_gate, w_up, w_down, norm_ple, w_ple_gate, w_ple_proj, norm_final):
    B, S, _ = x.shape
    pos = jnp.arange(S, dtype=jnp.int32)
    for i in range(DEPTH):
        h = _rmsnorm(x, norm_mix[i])
        proj = h @ w_in[i]
        q, k, v, u, gate_b, gate_c, merge_a, merge_b = jnp.split(proj, SPLITS, axis=-1)
        q = _rope(q.reshape(B, S, N_Q_HEADS, HEAD_DIM), pos)
        k = _rope(k.reshape(B, S, N_KV_HEADS, HEAD_DIM), pos)
        v = v.reshape(B, S, N_KV_HEADS, HEAD_DIM)
        outs, lses = [], []
        for g, (window, dilation) in enumerate(ATTN_GROUPS):
            q_g = q[:, :, g * HEADS_PER_GROUP:(g + 1) * HEADS_PER_GROUP]
            o_g, l_g = _dilated_window_attention(q_g, k, v, window, dilation)
            outs.append(o_g)
            lses.append(l_g)
        mix_w = jax.nn.softmax(jnp.stack(lses, axis=0), axis=0)
        y_a = jnp.sum(mix_w[..., None].astype(v.dtype) * jnp.stack(outs, axis=0), axis=0).reshape(B, S, A_OUT)
        y_b = gate_b * _short_conv(gate_c * u, w_conv[i])
        merged = jax.nn.sigmoid(merge_a) * (y_a @ w_up_a[i]) + jax.nn.sigmoid(merge_b) * (y_b @ w_out_b[i])
        x = x + merged @ w_o[i]
        h2 = _rmsnorm(x, norm_ffn[i])
        x = x + _hierarchical_moe(h2, w_group[i], b_group[i], w_router[i], b_router[i],
                                  w_gate[i], w_up[i], w_down[i])
        h3 = _rmsnorm(x, norm_ple[i])
        x = x + jax.nn.sigmoid(h3 @ w_ple_gate[i]) * (p[i] @ w_ple_proj[i])
    return _rmsnorm(x, norm_final)
```

```python
import numpy as np
from contextlib import ExitStack
import concourse.bass as bass
import concourse.mybir as mybir
from concourse.bass_utils import run_bass_kernel_spmd

dt = mybir.dt
F32, BF16, I32, U32 = dt.float32, dt.bfloat16, dt.int32, dt.uint32
AF = mybir.ActivationFunctionType
ALU = mybir.AluOpType
AX = mybir.AxisListType

NCORE = 8
D = 4096
S = 8192
TOK = S // NCORE
HALO = 2048
NT = TOK // 128
NTH = (TOK + HALO) // 128
KC = D // 128
QC, KVC, CW = 1536, 512, 2048
INC = 16896
DE = 1536
NE = 32
CAP = 128
EPS = 1e-6
SCALE = 128 ** -0.5
NSLOT = 4
SLOT = 8192
NEG = -30000.0
GROUPS = ((128, 1), (512, 4), (2048, 16))


class Ev:
    __slots__ = ("sem", "val")

    def __init__(self, sem, val):
        self.sem, self.val = sem, val


class Prog:
    def __init__(self, nc, stack):
        self.nc, self.stack = nc, stack
        self.ops = {k: [] for k in ("pe", "act", "dve", "pool", "sp")}
        self.sems, self.cnt = {}, {}

    def _sem(self, key):
        if key not in self.sems:
            self.sems[key] = self.stack.enter_context(self.nc.semaphore(key))
            self.cnt[key] = 0
        return self.sems[key]

    def op(self, eng, fn, waits=(), sig=True, key=None, dma=False):
        ev = None
        sg = None
        if sig:
            key = key or ("c_" + eng)
            sem = self._sem(key)
            inc = 16 if dma else 1
            self.cnt[key] += inc
            ev = Ev(sem, self.cnt[key])
            sg = (sem, inc)
        ws = [w for w in waits if w is not None]
        self.ops[eng].append((fn, ws, sg))
        return ev

    def pe(self, fn, waits=(), sig=True):
        return self.op("pe", fn, waits, sig)

    def act(self, fn, waits=(), sig=True):
        return self.op("act", fn, waits, sig)

    def dve(self, fn, waits=(), sig=True):
        return self.op("dve", fn, waits, sig)

    def dma(self, q, out, in_, key, waits=()):
        return self.op(q, lambda e: e.dma_start(out=out, in_=in_), waits, True, key, True)

    def barrier(self, engs=("pe", "act", "dve", "sp")):
        evs = []
        for eng in ("pe", "act", "dve"):
            evs.append(self.op(eng, lambda e: e.drain()))
        for key, sem in self.sems.items():
            if key.startswith("ring") or key.startswith("c_"):
                continue
            if self.cnt[key] > 0:
                evs.append(Ev(sem, self.cnt[key]))
        for eng in engs:
            self.op(eng, lambda e: e.nop(), evs, sig=False)
        return evs

    def emit(self, block):
        engmap = {"pe": block.tensor, "act": block.scalar, "dve": block.vector,
                  "pool": block.gpsimd, "sp": block.sync}
        for name, deco in engmap.items():
            ops = self.ops[name]

            def body(e, ops=ops):
                seen = {}
                for fn, waits, sg in ops:
                    for w in waits:
                        k = id(w.sem)
                        if seen.get(k, -1) >= w.val:
                            continue
                        e.wait_ge(w.sem, w.val)
                        seen[k] = w.val
                    ins = fn(e)
                    if sg is not None:
                        ins.then_inc(sg[0], sg[1])

            deco(body)


class Arena:
    def __init__(self, t, nbytes):
        self.t, self.n, self.p = t, nbytes, 0

    def alloc(self, nbytes, dtype=F32):
        req = nbytes
        nbytes = (nbytes + 63) // 64 * 64
        assert self.p + nbytes <= self.n, ("arena overflow", self.p, nbytes, self.n)
        a = self.t[:, self.p // 4:(self.p + req) // 4]
        self.p += nbytes
        return a if dtype == F32 else a.bitcast(dtype)

    def mark(self):
        return self.p

    def release(self, m):
        self.p = m


class Ring:
    def __init__(self, P, slots, specs):
        self.P, self.slots = P, slots
        self.specs = specs
        self.rec = []
        self.issued = 0
        self.loaded = {}
        self.freed = {}
        self.first_wait = None

    def _issue(self, j, spec):
        s = j % NSLOT
        waits = [self.freed.get(j - NSLOT), self.first_wait if j < NSLOT else None]
        evs = []
        for (dst_fn, src_fn) in spec:
            dst = dst_fn(self.slots[s])
            evs.append(self.P.dma("pool", dst, src_fn(), "ring%d" % s, waits))
        self.loaded[j] = evs[-1]
        self.issued = j + 1

    def get(self, spec):
        j = len(self.rec)
        self.rec.append(spec)
        if self.specs is None:
            self._issue(j, spec)
        else:
            hi = min(len(self.specs), j + NSLOT)
            while self.issued < hi:
                jj = self.issued
                if jj - NSLOT >= 0 and (jj - NSLOT) not in self.freed:
                    break
                self._issue(jj, self.specs[jj])
            assert j in self.loaded, ("ring deadlock", j)
        return j, self.slots[j % NSLOT], self.loaded[j]

    def done(self, j, ev):
        self.freed[j] = ev


def _build(specs, stop, debug):
    nc = bass.Bass("TRN2", target_bir_lowering=False)
    DR = {}

    def din(name, shape, dtype=F32):
        DR[name] = nc.dram_tensor(name, list(shape), dtype, kind="ExternalInput").ap()

    def dscr(name, shape, dtype=F32):
        DR[name] = nc.dram_tensor(name, list(shape), dtype, kind="Internal").ap()

    din("xh", [TOK + HALO, D])
    din("pp", [TOK, 256])
    din("cosF", [TOK + HALO, 512])
    din("sinF", [TOK + HALO, 512])
    din("km", [1, TOK + HALO])
    din("band", [128, 256])
    din("ident", [128, 128])
    din("tri", [128, 128])
    din("gcols", [128, 3 * KC])
    din("g_fin", [1, D])
    din("wconv", [128, 48])
    din("w_in", [D, INC])
    din("w_up_a", [512, D])
    din("w_out_b", [CW, D])
    din("w_o", [D, D])
    din("w_rt", [D, 36])
    din("b_rt", [1, 36])
    ne_decl = NE if stop >= 7 else 1
    din("w_gate", [ne_decl, D, DE])
    din("w_up", [ne_decl, D, DE])
    din("w_down", [ne_decl, DE, D])
    din("w_ple_gate", [D, D])
    din("w_ple_proj", [256, D])
    out_d = nc.dram_tensor("out", [TOK, D], F32, kind="ExternalOutput").ap()
    dscr("v_scr", [TOK + HALO, 512], BF16)
    dscr("att_scr", [TOK, 3 * 520])
    if debug:
        dbg_d = nc.dram_tensor("dbg", [4096, D], F32, kind="ExternalOutput").ap()

    with ExitStack() as stack:
        arena_t = stack.enter_context(nc.sbuf_tensor("arena", [128, 206 * 256], F32))
        psum = stack.enter_context(nc.psum_tensor("psum", [128, 8 * 512], F32))
        block = stack.enter_context(nc.Block())
        P = Prog(nc, stack)
        A = Arena(arena_t, 206 * 1024)

        def finish(dumps=()):
            evs = P.barrier(engs=("pe", "act", "dve", "sp", "pool"))
            last = []
            for (dst, src, q) in dumps:
                last.append(P.dma(q, dst, src, "fin_" + q))
            P.op("sp", lambda e: e.nop(), last, sig=False)
            P.op("pool", lambda e: e.nop(), last, sig=False)
            P.emit(block)
            return nc, ring.rec

        def bank(b, n=512, p=128):
            return psum[0:p, b * 512:b * 512 + n]

        def bankbf(b):
            return psum[:, b * 512:(b + 1) * 512].bitcast(BF16)

        ident_f = A.alloc(512)
        ident_b = A.alloc(256, BF16)
        band_b = A.alloc(512, BF16)
        kmb = A.alloc(2 * (TOK + HALO), BF16)
        ones_b = A.alloc(256, BF16)
        gcols = A.alloc(4 * 96)
        ring_flat = A.alloc(NSLOT * SLOT * 2, BF16)
        slots = [ring_flat[:, s * SLOT:(s + 1) * SLOT] for s in range(NSLOT)]
        hT_own = A.alloc(KC * (TOK + 2) * 2, BF16).rearrange("p (c n) -> p c n", c=KC)
        ring = Ring(P, slots, specs)

        e_c = []
        e_c.append(P.dma("sp", ident_f, DR["ident"], "c0"))
        e_c.append(P.dma("sp", gcols, DR["gcols"], "c0"))
        e_c.append(P.dma("pool", band_b, DR["band"], "c1"))
        e_c.append(P.dma("pool", kmb[0:1, :], DR["km"], "c1"))
        ev_c0, ev_c1 = e_c[1], e_c[3]
        ev_idb = P.dve(lambda e: e.tensor_copy(out=ident_b, in_=ident_f), [ev_c0])
        ev_ones = P.dve(lambda e: e.memset(ones_b, 1.0))

        m_stage = A.mark()
        kT = A.alloc(4 * (TOK + HALO) * 2, BF16).rearrange("p (h n) -> p h n", h=4)
        m1 = A.mark()
        xt = A.alloc(D * 4)
        hb = A.alloc(D * 2, BF16)
        hTt = A.alloc(KC * 128 * 2, BF16).rearrange("p (c n) -> p c n", c=KC)
        cst = A.alloc(2048)
        snt = A.alloc(2048)
        t1 = A.alloc(2048)
        t2 = A.alloc(2048)
        kb = A.alloc(1024, BF16)
        vb = A.alloc(1024, BF16)
        small = A.alloc(256)
        wkv = ring_flat.rearrange("p (c n) -> p c n", c=KC)
        ev_wkv = None
        for c4 in range(4):
            ev_wkv = P.dma("pool", wkv[:, c4 * 8:(c4 + 1) * 8, :],
                           DR["w_in"][c4 * 1024:(c4 + 1) * 1024, QC:QC + 1024].rearrange("(c p) n -> p c n", p=128),
                           "wkv")
        ev_xt_free = ev_cs_free = ev_hb_free = ev_hTt_free = None
        ev_bank_free = {}
        ev_kb_free = ev_vb_free = None
        ev_kT_last = None
        ev_s1_pe = None
        tp_i = 0
        for i in range(NTH):
            r0 = i * 128
            own = i >= NTH - NT
            ev_x = P.dma("sp", xt, DR["xh"][r0:r0 + 128, :], "xt", [ev_xt_free])
            ev_cs1 = P.dma("sp", cst[:, 0:512], DR["cosF"][r0:r0 + 128, :], "cs", [ev_cs_free])
            ev_cs2 = P.dma("sp", snt[:, 0:512], DR["sinF"][r0:r0 + 128, :], "cs", [ev_cs_free])
            ss = small[:, 0:1]
            e1 = P.act(lambda e, ss=ss: e.activation(out=hb, in_=xt, func=AF.Square, accum_out=ss),
                       [ev_x, ev_hb_free])
            e2 = P.dve(lambda e, ss=ss: e.tensor_scalar(out=small[:, 1:2], in0=ss, scalar1=1.0 / D, scalar2=EPS,
                                                        op0=ALU.mult, op1=ALU.add), [e1])
            e3 = P.act(lambda e: e.activation(out=small[:, 2:3], in_=small[:, 1:2], func=AF.Sqrt), [e2])
            e4 = P.dve(lambda e: e.reciprocal(out=small[:, 3:4], in_=small[:, 2:3]), [e3])
            e5 = P.act(lambda e: e.activation(out=hb, in_=xt, func=AF.Copy, scale=small[:, 3:4]), [e4])
            ev_xt_free = e5
            if own:
                off = 2 + (i - (NTH - NT)) * 128
                dst = hT_own[:, :, off:off + 128]
            else:
                dst = hTt
            ev_evs = []
            for q4 in range(4):
                b = tp_i % 2
                tp_i += 1
                pb = bankbf(b).rearrange("p (c n) -> p c n", c=8)
                for cc in range(8):
                    c = q4 * 8 + cc
                    evt = P.pe(lambda e, pb=pb, cc=cc, c=c: e.transpose(out=pb[:, cc, :], in_=hb[:, c * 128:(c + 1) * 128],
                                                                        identity=ident_b),
                               [e5, ev_idb, ev_bank_free.get(b), ev_hTt_free if not own else None], sig=(cc == 7))
                gsl = gcols[:, q4 * 8:(q4 + 1) * 8].unsqueeze(2).to_broadcast([128, 8, 128])
                eve = P.dve(lambda e, pb=pb, q4=q4, dst=dst, gsl=gsl: e.tensor_tensor(
                    out=dst[:, q4 * 8:(q4 + 1) * 8, :], in0=pb, in1=gsl, op=ALU.mult), [evt, ev_c0])
                ev_bank_free[b] = eve
                ev_evs.append(eve)
            ev_hb_free = evt
            ev_hT = ev_evs[-1]
            if i == NTH - NT - 1:
                ev_hT = P.dve(lambda e: e.tensor_copy(out=hT_own[:, :, 0:2], in_=hTt[:, :, 126:128]), [ev_hT])
            for nb in range(2):
                for c in range(KC):
                    lhsT = dst[:, c, :]
                    evm = P.pe(lambda e, nb=nb, c=c, lhsT=lhsT: e.matmul(
                        bank(2 + nb), lhsT=lhsT, rhs=wkv[:, c, nb * 512:(nb + 1) * 512],
                        start=(c == 0), stop=(c == KC - 1)),
                        [ev_hT, ev_wkv, ev_bank_free.get(2 + nb)], sig=(c == KC - 1))
                if nb == 0:
                    ev_k = evm
                else:
                    ev_v = evm
            if not own:
                ev_hTt_free = ev_v
            ev_s1_pe = ev_v
            k4 = bank(2).rearrange("p (h t d) -> p h t d", h=4, t=2)
            c4v = cst[:, 0:512].rearrange("p (h t d) -> p h t d", h=4, t=2)
            s4v = snt[:, 0:512].rearrange("p (h t d) -> p h t d", h=4, t=2)
            t24 = t2[:, 0:512].rearrange("p (h t d) -> p h t d", h=4, t=2)
            r1 = P.dve(lambda e: e.tensor_tensor(out=t1[:, 0:512], in0=bank(2), in1=cst[:, 0:512], op=ALU.mult),
                       [ev_k, ev_cs2])
            r2 = P.dve(lambda e, k4=k4, s4v=s4v, t24=t24: e.tensor_tensor(out=t24[:, :, 0, :], in0=k4[:, :, 1, :],
                                                                         in1=s4v[:, :, 0, :], op=ALU.mult), [r1])
            r3 = P.dve(lambda e, k4=k4, s4v=s4v, t24=t24: e.tensor_tensor(out=t24[:, :, 1, :], in0=k4[:, :, 0, :],
                                                                         in1=s4v[:, :, 1, :], op=ALU.mult), [r2])
            r4 = P.dve(lambda e: e.tensor_tensor(out=kb, in0=t1[:, 0:512], in1=t2[:, 0:512], op=ALU.add),
                       [r3, ev_kb_free])
            ev_bank_free[2] = r3
            ev_cs_free = r3
            rv = P.act(lambda e: e.activation(out=vb, in_=bank(3), func=AF.Copy), [ev_v, ev_vb_free])
            ev_bank_free[3] = rv
            ev_vb_free = P.dma("sp", DR["v_scr"][r0:r0 + 128, :], vb, "vst", [rv])
            if False:
                o0 = (i - (NTH - NT)) * 128
                ev_kb_free_d = P.dma("pool", dbg_d[o0:o0 + 128, 512:1024], kb, "dbgk", [r4])
                ev_vb_free = P.dma("pool", dbg_d[o0:o0 + 128, 1024:1536], vb, "vst", [rv])
            pb4 = bankbf(4)[:, 0:512].rearrange("p (h n) -> p h n", h=4)
            for h in range(4):
                evt = P.pe(lambda e, h=h, pb4=pb4: e.transpose(out=pb4[:, h, :], in_=kb[:, h * 128:(h + 1) * 128],
                                                               identity=ident_b),
                           [r4, ev_bank_free.get(4)], sig=(h == 3))
            ev_kb_free = evt
            ev_kT_last = P.act(lambda e, pb4=pb4, r0=r0: e.activation(out=kT[:, :, r0:r0 + 128], in_=pb4, func=AF.Copy),
                               [evt])
            ev_bank_free[4] = ev_kT_last
        ev_vst_all = ev_vb_free
        ring.first_wait = ev_s1_pe
        P.barrier()
        A.release(m1)

        qT = A.alloc(12 * TOK * 2, BF16).rearrange("p (h n) -> p h n", h=12)
        m2 = A.mark()
        cst_q = A.alloc(2048)
        snt_q = A.alloc(2048)
        t1_q = A.alloc(2048)
        t2_q = A.alloc(2048)
        qb = A.alloc(1024, BF16)
        ev_cs_free = None
        ev_qb_free = None
        ev_qT_last = None
        gi = 0
        for cb in range(3):
            jobs = []
            for half in range(2):
                c0 = cb * 512

                def src(half=half, c0=c0):
                    return DR["w_in"][half * 2048:(half + 1) * 2048, c0:c0 + 512].rearrange("(c p) n -> p c n", p=128)

                def dstf(slot):
                    return slot.rearrange("p (c n) -> p c n", c=16)
                jobs.append(ring.get([(dstf, src)]))
            for t in range(NT):
                r0 = HALO + t * 128
                ev_cs1 = P.dma("sp", cst_q[:, 0:512], DR["cosF"][r0:r0 + 128, :], "cs", [ev_cs_free])
                ev_cs2 = P.dma("sp", snt_q[:, 0:512], DR["sinF"][r0:r0 + 128, :], "cs", [ev_cs_free])
                b = 2 + gi % 2
                gi += 1
                for c in range(KC):
                    j, slot, evl = jobs[c // 16]
                    sv = slot.rearrange("p (c n) -> p c n", c=16)
                    evm = P.pe(lambda e, b=b, c=c, sv=sv, t=t: e.matmul(
                        bank(b), lhsT=hT_own[:, c, 2 + t * 128:2 + (t + 1) * 128], rhs=sv[:, c % 16, :],
                        start=(c == 0), stop=(c == KC - 1)), [evl, ev_bank_free.get(b)], sig=(c == KC - 1))
                k4 = bank(b).rearrange("p (h t d) -> p h t d", h=4, t=2)
                s4v = snt_q[:, 0:512].rearrange("p (h t d) -> p h t d", h=4, t=2)
                t24 = t2_q[:, 0:512].rearrange("p (h t d) -> p h t d", h=4, t=2)
                r1 = P.dve(lambda e, b=b: e.tensor_tensor(out=t1_q[:, 0:512], in0=bank(b), in1=cst_q[:, 0:512], op=ALU.mult),
                           [evm, ev_cs2])
                r2 = P.dve(lambda e, k4=k4, s4v=s4v, t24=t24: e.tensor_tensor(out=t24[:, :, 0, :], in0=k4[:, :, 1, :],
                                                                             in1=s4v[:, :, 0, :], op=ALU.mult), [r1])
                r3 = P.dve(lambda e, k4=k4, s4v=s4v, t24=t24: e.tensor_tensor(out=t24[:, :, 1, :], in0=k4[:, :, 0, :],
                                                                             in1=s4v[:, :, 1, :], op=ALU.mult), [r2])
                r4 = P.dve(lambda e: e.tensor_tensor(out=qb, in0=t1_q[:, 0:512], in1=t2_q[:, 0:512], op=ALU.add),
                           [r3, ev_qb_free])
                ev_bank_free[b] = r3
                ev_cs_free = r3
                pb4 = bankbf(4)[:, 0:512].rearrange("p (h n) -> p h n", h=4)
                for h in range(4):
                    evt = P.pe(lambda e, h=h, pb4=pb4: e.transpose(out=pb4[:, h, :], in_=qb[:, h * 128:(h + 1) * 128],
                                                                   identity=ident_b),
                               [r4, ev_bank_free.get(4)], sig=(h == 3))
                ev_qb_free = evt
                if False:
                    ev_qb_free = P.dma("pool", dbg_d[t * 128:(t + 1) * 128, 1536:2048], qb, "dbgq", [r4, evt])
                ev_qT_last = P.act(lambda e, pb4=pb4, cb=cb, t=t: e.activation(
                    out=qT[:, cb * 4:(cb + 1) * 4, t * 128:(t + 1) * 128], in_=pb4, func=AF.Copy), [evt])
                ev_bank_free[4] = ev_qT_last
                if t == NT - 1:
                    for (j, slot, evl) in jobs:
                        ring.done(j, evm)
        P.barrier()
        A.release(m2)

        y_aT = A.alloc(4 * TOK * 2, BF16).rearrange("p (h n) -> p h n", h=4)
        m3 = A.mark()
        vs = [A.alloc(2 * 512 * 2, BF16).rearrange("p (k n) -> p k n", k=2) for _ in range(2)]
        e_b = A.alloc(2048, BF16).rearrange("p (h n) -> p h n", h=4)
        eT_full = A.alloc(2048, BF16)
        eT = eT_full.rearrange("p (h k n) -> p h k n", h=4, k=2)
        ob = [A.alloc(4 * 130 * 4).rearrange("p (h n) -> p h n", h=4) for _ in range(2)]
        st = A.alloc(256)
        ev_vs_free = [None, None]
        ev_ob_free = [None, None]
        ev_e_free = ev_eT_free = None
        ev_S_free = None
        ev_O_free = None
        ev_st_free = None
        ev_T_free = ev_bank_free.get(0)
        ev_att_last = None
        bi = 0
        BT, BO = 0, 7
        Tb_full = bankbf(BT)
        Tb = Tb_full.rearrange("p (h k n) -> p h k n", h=4, k=2)
        for g, (win, d) in enumerate(GROUPS):
            ncls = TOK // d
            nq = min(128, ncls)
            nblk = max(1, ncls // 128)
            nk = nq + 128
            kbs = [(0, 128), (128, nk - 128)]
            for r in range(d):
                for qblk in range(nblk):
                    l0 = qblk * 128
                    qs = l0 * d + r
                    ks = HALO + (l0 - 128) * d + r
                    vi = bi % 2
                    bi += 1
                    vv = vs[vi]
                    evv = None
                    for kbi, (ko, kn) in enumerate(kbs):
                        a0 = ks + ko * d
                        evv = P.dma("sp", vv[0:kn, kbi, :], DR["v_scr"][a0:a0 + (kn - 1) * d + 1:d, :], "vs%d" % vi,
                                    [ev_vs_free[vi], ev_vst_all])
                    obv = ob[vi]
                    S4 = psum[0:nq, 5 * 512:7 * 512].rearrange("p (h n) -> p h n", h=4)
                    evS = None
                    for hh in range(4):
                        qsl = qT[:, g * 4 + hh, qs:qs + (nq - 1) * d + 1:d]
                        ksl = kT[:, hh, ks:ks + (nk - 1) * d + 1:d]
                        kmsl = kmb[0:1, ks:ks + (nk - 1) * d + 1:d]
                        Sb = S4[:, hh, 0:nk]
                        P.pe(lambda e, Sb=Sb, qsl=qsl, ksl=ksl: e.matmul(Sb, lhsT=qsl, rhs=ksl, start=True, stop=False),
                             [ev_qT_last, ev_kT_last, ev_S_free], sig=False)
                        P.pe(lambda e, Sb=Sb, nq=nq, nk=nk: e.matmul(Sb, lhsT=ident_b[0:nq, 0:nq], rhs=band_b[0:nq, 0:nk],
                                                                    start=False, stop=False), [ev_c1, ev_idb], sig=False)
                        evS = P.pe(lambda e, Sb=Sb, nq=nq, kmsl=kmsl: e.matmul(Sb, lhsT=ones_b[0:1, 0:nq], rhs=kmsl,
                                                                              start=False, stop=True), [ev_ones], sig=(hh == 3))
                    S4k = S4[:, :, 0:nk]
                    mx = st[0:nq, 0:4]
                    nmx = st[0:nq, 4:8]
                    e_mx = P.dve(lambda e, S4k=S4k, mx=mx: e.reduce_max(out=mx, in_=S4k, axis=AX.X), [evS, ev_st_free])
                    e_nm = P.dve(lambda e, mx=mx, nmx=nmx: e.tensor_scalar(out=nmx, in0=mx, scalar1=-SCALE, scalar2=None,
                                                                          op0=ALU.mult), [e_mx])
                    e_m2 = P.dve(lambda e, mx=mx, obv=obv, nq=nq: e.tensor_scalar(
                        out=obv[0:nq, :, 128], in0=mx, scalar1=SCALE, scalar2=None, op0=ALU.mult),
                        [e_mx, ev_ob_free[vi]])
                    e_ex = None
                    for hh in range(4):
                        e_ex = P.act(lambda e, S4=S4, nq=nq, nk=nk, st=st, obv=obv, hh=hh: e.activation(
                            out=e_b[0:nq, hh, 0:nk], in_=S4[:, hh, 0:nk], func=AF.Exp, bias=st[0:nq, 4 + hh:5 + hh], scale=SCALE,
                            accum_out=obv[0:nq, hh, 129:130]), [e_nm, ev_e_free, ev_ob_free[vi]])
                    ev_S_free = e_ex
                    ev_st_free = e_ex
                    evt = None
                    for hh in range(4):
                        for kbi, (ko, kn) in enumerate(kbs):
                            evt = P.pe(lambda e, hh=hh, kbi=kbi, ko=ko, kn=kn, nq=nq: e.transpose(
                                out=Tb[0:kn, hh, kbi, 0:nq], in_=e_b[0:nq, hh, ko:ko + kn], identity=ident_b[0:nq, 0:nq]),
                                [e_ex, ev_T_free], sig=(hh == 3 and kbi == 1))
                    ev_e_free = evt
                    evc = P.dve(lambda e: e.tensor_copy(out=eT_full, in_=Tb_full), [evt, ev_eT_free])
                    ev_T_free = evc
                    Ob = bank(BO, 512, nq)
                    evo = None
                    for hh in range(4):
                        for kbi, (ko, kn) in enumerate(kbs):
                            evo = P.pe(lambda e, Ob=Ob, kbi=kbi, kn=kn, nq=nq, hh=hh, vv=vv: e.matmul(
                                Ob[:, hh * 128:(hh + 1) * 128], lhsT=eT[0:kn, hh, kbi, 0:nq],
                                rhs=vv[0:kn, kbi, hh * 128:(hh + 1) * 128], start=(kbi == 0), stop=(kbi == 1)),
                                [evc, evv, ev_O_free], sig=(hh == 3 and kbi == 1))
                    ev_eT_free = evo
                    ev_vs_free[vi] = evo
                    e_oc = P.dve(lambda e, Ob=Ob, obv=obv, nq=nq: e.tensor_copy(
                        out=obv[0:nq, :, 0:128], in_=Ob.rearrange("p (h n) -> p h n", h=4)),
                        [evo, ev_ob_free[vi]])
                    ev_O_free = e_oc
                    dst = DR["att_scr"][qs:qs + (nq - 1) * d + 1:d, g * 520:(g + 1) * 520]
                    ev_ob_free[vi] = P.dma("sp", dst, obv[0:nq, :, :].rearrange("p h n -> p (h n)"), "ob%d" % vi,
                                           [e_oc, e_ex, e_m2])
                    ev_att_last = ev_ob_free[vi]
        P.barrier()
        A.release(m3)
        at = A.alloc(3 * 520 * 4).rearrange("p (g h n) -> p g h n", g=3, h=4)
        cw = A.alloc(256)
        ya = A.alloc(2048)
        yab = A.alloc(1024, BF16)
        ev_at_free = None
        ev_yab_free = None
        ev_ya_last = None
        ev_ob_all = [ev_ob_free[0], ev_ob_free[1]]
        for t in range(NT):
            ev_l = P.dma("sp", at.rearrange("p g h n -> p (g h n)"), DR["att_scr"][t * 128:(t + 1) * 128, :], "at",
                         [ev_at_free] + ev_ob_all)
            mv = at[:, :, :, 128]
            sv_ = at[:, :, :, 129]
            M = cw[:, 0:4]
            c1 = P.dve(lambda e, M=M, mv=mv: e.tensor_tensor(out=M, in0=mv[:, 0, :], in1=mv[:, 1, :], op=ALU.max), [ev_l])
            c2 = P.dve(lambda e, M=M, mv=mv: e.tensor_tensor(out=M, in0=M, in1=mv[:, 2, :], op=ALU.max), [c1])
            wv = cw[:, 4:16].rearrange("p (g h) -> p g h", g=3)
            c3 = P.dve(lambda e, M=M, mv=mv, wv=wv: e.tensor_tensor(
                out=wv, in0=mv, in1=M.unsqueeze(1).to_broadcast([128, 3, 4]), op=ALU.subtract), [c2])
            c4 = P.act(lambda e, wv=wv: e.activation(out=wv, in_=wv, func=AF.Exp), [c3])
            ws = cw[:, 16:28].rearrange("p (g h) -> p g h", g=3)
            c5 = P.dve(lambda e, ws=ws, wv=wv, sv_=sv_: e.tensor_tensor(out=ws, in0=wv, in1=sv_, op=ALU.mult), [c4])
            den = cw[:, 28:32]
            c6 = P.dve(lambda e, den=den, ws=ws: e.tensor_tensor(out=den, in0=ws[:, 0, :], in1=ws[:, 1, :], op=ALU.add), [c5])
            c7 = P.dve(lambda e, den=den, ws=ws: e.tensor_tensor(out=den, in0=den, in1=ws[:, 2, :], op=ALU.add), [c6])
            c8 = P.dve(lambda e, den=den: e.reciprocal(out=den, in_=den), [c7])
            c9 = P.dve(lambda e, den=den, wv=wv: e.tensor_tensor(
                out=wv, in0=wv, in1=den.unsqueeze(1).to_broadcast([128, 3, 4]), op=ALU.mult), [c8])
            prev = c9
            ya4 = ya[:, 0:512].rearrange("p (h n) -> p h n", h=4)
            for hh in range(4):
                p0 = P.dve(lambda e, hh=hh, ya4=ya4, wv=wv: e.tensor_scalar(
                    out=ya4[:, hh, :], in0=at[:, 0, hh, 0:128], scalar1=wv[:, 0, hh:hh + 1], scalar2=None, op0=ALU.mult),
                    [prev, ev_ya_last])
                p1 = P.dve(lambda e, hh=hh, ya4=ya4, wv=wv: e.scalar_tensor_tensor(
                    out=ya4[:, hh, :], in0=at[:, 1, hh, 0:128], scalar=wv[:, 1, hh:hh + 1], in1=ya4[:, hh, :],
                    op0=ALU.mult, op1=ALU.add), [p0])
                prev = P.dve(lambda e, hh=hh, ya4=ya4, wv=wv: e.scalar_tensor_tensor(
                    out=ya4[:, hh, :], in0=at[:, 2, hh, 0:128], scalar=wv[:, 2, hh:hh + 1], in1=ya4[:, hh, :],
                    op0=ALU.mult, op1=ALU.add), [p1])
            ev_at_free = prev
            evb = P.dve(lambda e: e.tensor_copy(out=yab, in_=ya[:, 0:512]), [prev, ev_yab_free])
            ev_ya_last = evb
            if False:
                pass
            pb4 = bankbf(4)[:, 0:512].rearrange("p (h n) -> p h n", h=4)
            for h in range(4):
                evt = P.pe(lambda e, h=h, pb4=pb4: e.transpose(out=pb4[:, h, :], in_=yab[:, h * 128:(h + 1) * 128],
                                                               identity=ident_b), [evb, ev_bank_free.get(4)], sig=(h == 3))
            ev_yab_free = evt
            ev_yaT = P.act(lambda e, pb4=pb4, t=t: e.activation(out=y_aT[:, :, t * 128:(t + 1) * 128], in_=pb4,
                                                                func=AF.Copy), [evt])
            ev_bank_free[4] = ev_yaT

        if stop == 3:
            return finish([(dbg_d[h * 128:(h + 1) * 128, 0:TOK], y_aT[:, h, :], "pool") for h in range(4)])
        P.barrier()
        A.release(m_stage)
        y_aT2 = A.alloc(4 * TOK * 2, BF16).rearrange("p (h n) -> p h n", h=4)
        ev_cp = P.dve(lambda e: e.tensor_copy(out=y_aT2, in_=y_aT))
        P.barrier()
        y_bT = A.alloc(16 * TOK * 2, BF16).rearrange("p (c n) -> p c n", c=16)
        wcv = A.alloc(192)
        ev_wcv = P.dma("sp", wcv, DR["wconv"], "c0")
        m4 = A.mark()

        def slot16(slot):
            return slot.rearrange("p (c n) -> p c n", c=16)

        def w_jobs(name, r0, nrow, c0, ncol=512):
            jobs = []
            nkc = nrow // 128
            for k0 in range(0, nkc, 16):
                kn = min(16, nkc - k0)

                def src(k0=k0, kn=kn):
                    return DR[name][r0 + k0 * 128:r0 + (k0 + kn) * 128, c0:c0 + ncol].rearrange("(c p) n -> p c n", p=128)

                def dstf(slot, kn=kn):
                    return slot[:, 0:kn * ncol].rearrange("p (c n) -> p c n", c=kn)
                j, slot, evl = ring.get([(dstf, src)])
                jobs.append((j, slot[:, 0:kn * ncol].rearrange("p (c n) -> p c n", c=kn), evl, k0, kn))
            return jobs

        def job_for(jobs, c):
            for jb in jobs:
                if jb[3] <= c < jb[3] + jb[4]:
                    return jb
            raise AssertionError

        bfree = {}

        def fm_group(jobs, sub, nkc, rhs_fn, banks, halo_bank=None):
            evs = []
            for half in range(2):
                b = banks[half]
                for c in range(nkc):
                    jb = job_for(jobs, c)
                    ev = P.pe(lambda e, b=b, jb=jb, c=c, half=half: e.matmul(
                        bank(b), lhsT=jb[1][:, c - jb[3], sub * 128:(sub + 1) * 128], rhs=rhs_fn(c, half),
                        start=(c == 0), stop=(c == nkc - 1)), [jb[2], bfree.get(b)], sig=(c == nkc - 1))
                evs.append(ev)
            if halo_bank is not None:
                for c in range(nkc):
                    jb = job_for(jobs, c)
                    ev = P.pe(lambda e, jb=jb, c=c: e.matmul(
                        bank(halo_bank, 2), lhsT=jb[1][:, c - jb[3], sub * 128:(sub + 1) * 128], rhs=hT_own[:, c, 0:2],
                        start=(c == 0), stop=(c == nkc - 1)), [jb[2], bfree.get(halo_bank)], sig=(c == nkc - 1))
                evs.append(ev)
            return evs

        def rhs_h(c, half):
            return hT_own[:, c, 2 + half * 512:2 + (half + 1) * 512]

        u_sb = [A.alloc(1026 * 4) for _ in range(4)]
        cu = A.alloc(1026 * 4)
        U0, B0, C0 = QC + 2 * KVC, QC + 2 * KVC + CW, QC + 2 * KVC + 2 * CW
        pairs = [(2, 3), (4, 5)]
        gidx = 0
        ev_u = [None] * 4
        ev_cu_free = None
        ev_usb_free = [None] * 4
        for J in range(4):
            jobs = w_jobs("w_in", 0, D, U0 + J * 512)
            for sub in range(4):
                bk = pairs[gidx % 2]
                gidx += 1
                evs = fm_group(jobs, sub, KC, rhs_h, bk, halo_bank=6)
                us = u_sb[sub]
                for half in range(2):
                    ev = P.act(lambda e, us=us, half=half, bk=bk: e.activation(
                        out=us[:, 2 + half * 512:2 + (half + 1) * 512], in_=bank(bk[half]), func=AF.Copy),
                        [evs[half], ev_usb_free[sub]])
                    bfree[bk[half]] = ev
                ev = P.act(lambda e, us=us: e.activation(out=us[:, 0:2], in_=bank(6, 2), func=AF.Copy), [evs[2]])
                bfree[6] = ev
                ev_u[sub] = ev
            for jb in jobs:
                ring.done(jb[0], evs[2])
            jobs = w_jobs("w_in", 0, D, C0 + J * 512)
            for sub in range(4):
                j = J * 4 + sub
                bk = pairs[gidx % 2]
                gidx += 1
                evs = fm_group(jobs, sub, KC, rhs_h, bk, halo_bank=6)
                us = u_sb[sub]
                e3 = None
                for half in range(2):
                    e3 = P.dve(lambda e, us=us, half=half, bk=bk: e.tensor_tensor(
                        out=cu[:, 2 + half * 512:2 + (half + 1) * 512], in0=bank(bk[half]),
                        in1=us[:, 2 + half * 512:2 + (half + 1) * 512], op=ALU.mult),
                        [evs[half], ev_u[sub], ev_cu_free])
                    bfree[bk[half]] = e3
                e4 = P.dve(lambda e, us=us: e.tensor_tensor(out=cu[:, 0:2], in0=bank(6, 2), in1=us[:, 0:2], op=ALU.mult),
                           [evs[2], e3])
                bfree[6] = e4
                c1 = P.dve(lambda e, us=us, j=j: e.tensor_scalar(out=us[:, 0:1024], in0=cu[:, 0:1024],
                                                                 scalar1=wcv[:, j * 3:j * 3 + 1], scalar2=None, op0=ALU.mult),
                           [e4, ev_wcv])
                c2 = P.dve(lambda e, us=us, j=j: e.scalar_tensor_tensor(
                    out=us[:, 0:1024], in0=cu[:, 1:1025], scalar=wcv[:, j * 3 + 1:j * 3 + 2], in1=us[:, 0:1024],
                    op0=ALU.mult, op1=ALU.add), [c1])
                c3 = P.dve(lambda e, us=us, j=j: e.scalar_tensor_tensor(
                    out=us[:, 0:1024], in0=cu[:, 2:1026], scalar=wcv[:, j * 3 + 2:j * 3 + 3], in1=us[:, 0:1024],
                    op0=ALU.mult, op1=ALU.add), [c2])
                ev_cu_free = c3
                ev_u[sub] = c3
            for jb in jobs:
                ring.done(jb[0], evs[2])
            jobs = w_jobs("w_in", 0, D, B0 + J * 512)
            for sub in range(4):
                j = J * 4 + sub
                bk = pairs[gidx % 2]
                gidx += 1
                evs = fm_group(jobs, sub, KC, rhs_h, bk)
                us = u_sb[sub]
                for half in range(2):
                    ev = P.dve(lambda e, us=us, half=half, bk=bk, j=j: e.tensor_tensor(
                        out=y_bT[:, j, half * 512:(half + 1) * 512], in0=bank(bk[half]),
                        in1=us[:, half * 512:(half + 1) * 512], op=ALU.mult), [evs[half], ev_u[sub]])
                    bfree[bk[half]] = ev
                ev_usb_free[sub] = ev
            for jb in jobs:
                ring.done(jb[0], evs[1])
        if stop == 4:
            return finish([(dbg_d[c * 128:(c + 1) * 128, 0:TOK], y_bT[:, c, :], "pool") for c in range(16)])
        P.barrier()
        A.release(m4)

        dscr("mT_scr", [KC, 128, TOK], BF16)
        part = A.alloc(4 * TOK * 4).rearrange("p (s n) -> p s n", s=4)
        sg = A.alloc(2048)
        tmpm = A.alloc(2048)
        mT = [A.alloc(TOK * 2, BF16) for _ in range(2)]
        MA0, MB0 = QC + 2 * KVC + 3 * CW, QC + 2 * KVC + 3 * CW + D
        quads = [(2, 3, 4, 5), (0, 1, 6, 7)]
        ev_sg_free = None
        ev_tmp_free = None
        ev_mT_free = [None, None]
        ev_part = [None] * 4
        ev_part_free = [None] * 4
        mi = 0
        ev_mT_all = None
        for J in range(8):
            for phase in range(2):
                jobs_m = w_jobs("w_in", 0, D, (MA0 if phase == 0 else MB0) + J * 512)
                if phase == 0:
                    jobs_p = w_jobs("w_up_a", 0, 512, J * 512)
                    nkp = 4
                    rhs_p = lambda c, half: y_aT2[:, c, half * 512:(half + 1) * 512]
                else:
                    jobs_p = w_jobs("w_out_b", 0, CW, J * 512)
                    nkp = 16
                    rhs_p = lambda c, half: y_bT[:, c, half * 512:(half + 1) * 512]
                for sub in range(4):
                    j = J * 4 + sub
                    qd = quads[gidx % 2]
                    gidx += 1
                    evm = fm_group(jobs_m, sub, KC, rhs_h, qd[0:2])
                    evp = fm_group(jobs_p, sub, nkp, rhs_p, qd[2:4])
                    if phase == 1:
                        mt = mT[mi % 2]
                        mfree = ev_mT_free[mi % 2]
                    for half in range(2):
                        s1 = P.act(lambda e, qd=qd, half=half: e.activation(out=sg[:, 0:512], in_=bank(qd[half]),
                                                                            func=AF.Sigmoid), [evm[half], ev_sg_free])
                        bfree[qd[half]] = s1
                        if phase == 0:
                            s2 = P.dve(lambda e, qd=qd, half=half, sub=sub: e.tensor_tensor(
                                out=part[:, sub, half * 512:(half + 1) * 512], in0=bank(qd[2 + half]), in1=sg[:, 0:512],
                                op=ALU.mult), [s1, evp[half], ev_part_free[sub]])
                            bfree[qd[2 + half]] = s2
                            ev_sg_free = s2
                            ev_part[sub] = s2
                        else:
                            s2 = P.dve(lambda e, qd=qd, half=half: e.tensor_tensor(
                                out=tmpm[:, 0:512], in0=bank(qd[2 + half]), in1=sg[:, 0:512], op=ALU.mult),
                                [s1, evp[half], ev_tmp_free])
                            bfree[qd[2 + half]] = s2
                            ev_sg_free = s2
                            s3 = P.dve(lambda e, half=half, sub=sub, mt=mt: e.tensor_tensor(
                                out=mt[:, half * 512:(half + 1) * 512], in0=tmpm[:, 0:512],
                                in1=part[:, sub, half * 512:(half + 1) * 512], op=ALU.add), [s2, ev_part[sub], mfree])
                            ev_tmp_free = s3
                            ev_part_free[sub] = s3
                    if phase == 1:
                        ev_mT_free[mi % 2] = P.dma("sp", DR["mT_scr"][j], mt, "mT%d" % (mi % 2), [s3])
                        ev_mT_all = ev_mT_free[mi % 2]
                        mi += 1
                for jb in jobs_m:
                    ring.done(jb[0], evp[1])
                for jb in jobs_p:
                    ring.done(jb[0], evp[1])
        P.barrier()
        A.release(m_stage)

        dscr("x2_scr", [TOK, D])
        mTall = hT_own
        ev_ml = None
        for c in range(KC):
            ev_ml = P.dma("sp", mTall[:, c, 2:2 + TOK], DR["mT_scr"][c], "mload")
        xs = [A.alloc(2048) for _ in range(2)]
        ev_xs_free = [None, None]
        m5 = A.mark()

        def tm_gemm(jobs_list, nkcs, lhs_fns, banks_sets, epilogue, ntiles=NT):
            last = None
            for t in range(ntiles):
                evs = []
                bks = banks_sets[t % 2]
                for i, (jobs, nkc, lf) in enumerate(zip(jobs_list, nkcs, lhs_fns)):
                    b = bks[i]
                    for c in range(nkc):
                        jb = job_for(jobs, c)
                        ev = P.pe(lambda e, b=b, jb=jb, c=c, lf=lf, t=t: e.matmul(
                            bank(b), lhsT=lf(c, t), rhs=jb[1][:, c - jb[3], :], start=(c == 0), stop=(c == nkc - 1)),
                            [jb[2], bfree.get(b), ev_ml], sig=(c == nkc - 1))
                    evs.append(ev)
                epilogue(t, bks, evs)
                last = evs[-1]
            for jobs in jobs_list:
                for jb in jobs:
                    ring.done(jb[0], last)

        xi = [0]

        def resid_epilogue(src_name, src_row0, dst_name, cb):
            def ep(t, bks, evs):
                k = xi[0] % 2
                xi[0] += 1
                xb = xs[k]
                l = P.dma("sp", xb[:, 0:512], DR[src_name][src_row0 + t * 128:src_row0 + (t + 1) * 128, cb * 512:(cb + 1) * 512],
                          "xs%d" % k, [ev_xs_free[k]])
                a = P.dve(lambda e, xb=xb, b=bks[0]: e.tensor_tensor(out=xb[:, 0:512], in0=bank(b), in1=xb[:, 0:512], op=ALU.add),
                          [l, evs[0]])
                bfree[bks[0]] = a
                ev_xs_free[k] = P.dma("sp", DR[dst_name][t * 128:(t + 1) * 128, cb * 512:(cb + 1) * 512], xb[:, 0:512],
                                      "xs%d" % k, [a])
            return ep

        for cb in range(8):
            jobs = w_jobs("w_o", 0, D, cb * 512)
            tm_gemm([jobs], [KC], [lambda c, t: mTall[:, c, 2 + t * 128:2 + (t + 1) * 128]], [(2,), (3,)],
                    resid_epilogue("xh", HALO, "x2_scr", cb))
        if stop == 5:
            return finish([(dbg_d[0:TOK, :], DR["x2_scr"], "sp")])
        bar5 = P.barrier()
        A.release(m5)

        dscr("h2_scr", [NE * CAP, D], BF16)
        dscr("y_scr", [NE * CAP, D])
        wrt = A.alloc(KC * 36 * 4).rearrange("p (c n) -> p c n", c=KC)
        brt = A.alloc(36 * 4)
        ecap = A.alloc(128)
        tri_f = A.alloc(512)
        ones_f = A.alloc(512)
        cntb = A.alloc(128)
        slots_i = A.alloc(NT * 2 * 4, I32)
        wts = A.alloc(NT * 2 * 4)
        m6 = A.mark()
        x2t = A.alloc(D * 4)
        h2Tf = A.alloc(KC * 128 * 4).rearrange("p (c n) -> p c n", c=KC)
        hbb = A.alloc(D * 2, BF16)
        rs = A.alloc(512)
        ev_k = [P.dma("sp", wrt, DR["w_rt"].rearrange("(c p) n -> p c n", p=128), "c0"),
                P.dma("sp", brt, DR["b_rt"][0, :].partition_broadcast(128), "c0"),
                P.dma("sp", tri_f, DR["tri"], "c0")]
        ev_k6 = ev_k[-1]
        ev_k6b = P.dve(lambda e: e.memset(ones_f, 1.0))
        ev_k6c = P.dve(lambda e: e.memset(cntb, 0.0))
        ev_k6d = P.op("pool", lambda e: e.iota(ecap, pattern=[[CAP, NE]], base=0, channel_multiplier=0,
                                              allow_small_or_imprecise_dtypes=True), [])
        ev_x2_free = ev_hbb_free = ev_h2Tf_free = None
        ev_cnt = ev_k6c
        ev_rs_free = None
        ev_sc_all = []
        for t in range(NT):
            lx = P.dma("sp", x2t, DR["x2_scr"][t * 128:(t + 1) * 128, :], "x2t", [ev_x2_free])
            a1 = P.act(lambda e: e.activation(out=hbb, in_=x2t, func=AF.Square, accum_out=rs[:, 0:1]), [lx, ev_hbb_free, ev_rs_free])
            a2 = P.dve(lambda e: e.tensor_scalar(out=rs[:, 1:2], in0=rs[:, 0:1], scalar1=1.0 / D, scalar2=EPS,
                                                 op0=ALU.mult, op1=ALU.add), [a1])
            a3 = P.act(lambda e: e.activation(out=rs[:, 2:3], in_=rs[:, 1:2], func=AF.Sqrt), [a2])
            a4 = P.dve(lambda e: e.reciprocal(out=rs[:, 3:4], in_=rs[:, 2:3]), [a3])
            a5 = P.act(lambda e: e.activation(out=x2t, in_=x2t, func=AF.Copy, scale=rs[:, 3:4]), [a4])
            a6 = P.dve(lambda e: e.tensor_copy(out=hbb, in_=x2t), [a5])
            evE = None
            for q8 in range(8):
                b = q8 % 2
                pb = bank(b).rearrange("p (c n) -> p c n", c=4)
                for cc in range(4):
                    c = q8 * 4 + cc
                    evt = P.pe(lambda e, pb=pb, cc=cc, c=c: e.transpose(out=pb[:, cc, :], in_=x2t[:, c * 128:(c + 1) * 128],
                                                                        identity=ident_f), [a5, bfree.get(b), ev_h2Tf_free],
                               sig=(cc == 3))
                gsl = gcols[:, KC + q8 * 4:KC + (q8 + 1) * 4].unsqueeze(2).to_broadcast([128, 4, 128])
                evE = P.dve(lambda e, pb=pb, q8=q8, gsl=gsl: e.tensor_tensor(out=h2Tf[:, q8 * 4:(q8 + 1) * 4, :], in0=pb,
                                                                             in1=gsl, op=ALU.mult), [evt])
                bfree[b] = evE
            ev_x2_free = evt
            for c in range(KC):
                evl = P.pe(lambda e, c=c: e.matmul(bank(2, 36), lhsT=h2Tf[:, c, :], rhs=wrt[:, c, :],
                                                   start=(c == 0), stop=(c == KC - 1)),
                           [evE, ev_k6, bfree.get(2)], sig=(c == KC - 1))
            ev_h2Tf_free = evl
            lg = rs[:, 8:44]
            r0_ = P.dve(lambda e: e.tensor_tensor(out=lg, in0=bank(2, 36), in1=brt, op=ALU.add), [evl, ev_k6])
            bfree[2] = r0_
            gmax = rs[:, 4:5]
            r1_ = P.dve(lambda e: e.reduce_max(out=gmax, in_=rs[:, 8:12], axis=AX.X), [r0_])
            ohg = rs[:, 44:48]
            r2_ = P.dve(lambda e: e.tensor_scalar(out=ohg, in0=rs[:, 8:12], scalar1=gmax, scalar2=None, op0=ALU.is_equal), [r1_])
            ngm = rs[:, 5:6]
            r3_ = P.dve(lambda e: e.tensor_scalar(out=ngm, in0=gmax, scalar1=-1.0, scalar2=None, op0=ALU.mult), [r1_])
            r4_ = P.act(lambda e: e.activation(out=rs[:, 48:52], in_=rs[:, 8:12], func=AF.Exp, bias=ngm, scale=1.0,
                                               accum_out=rs[:, 6:7]), [r3_])
            r5_ = P.dve(lambda e: e.reciprocal(out=rs[:, 7:8], in_=rs[:, 6:7]), [r4_])
            pen = rs[:, 52:56]
            r6_ = P.dve(lambda e: e.tensor_scalar(out=pen, in0=ohg, scalar1=-1.0, scalar2=1.0e4, op0=ALU.add, op1=ALU.mult), [r2_])
            em = rs[:, 56:88]
            r7_ = P.dve(lambda e: e.tensor_tensor(out=em.rearrange("p (g k) -> p g k", g=4),
                                                  in0=rs[:, 12:44].rearrange("p (g k) -> p g k", g=4),
                                                  in1=pen.unsqueeze(2).to_broadcast([128, 4, 8]), op=ALU.add), [r6_])
            top8 = rs[:, 88:96]
            r8_ = P.dve(lambda e: e.max(out=top8, in_=em), [r7_])
            oh = [rs[:, 96:128], None]
            r9_ = P.dve(lambda e: e.tensor_scalar(out=rs[:, 96:128], in0=em, scalar1=top8[:, 0:1], scalar2=None,
                                                  op0=ALU.is_equal), [r8_])
            ev_sc_all.append((t, r0_, r5_, r8_, r9_))
            ev_rs_free = r9_
            rs2 = hbb.bitcast(F32) if False else None
            if t == 0:
                rq = A.alloc(1024)
            oh2 = rq[:, 0:32]
            q1 = P.dve(lambda e: e.tensor_scalar(out=oh2, in0=em, scalar1=top8[:, 1:2], scalar2=None, op0=ALU.is_equal), [r8_, ev_rs_free if False else None])
            dl = rq[:, 32:33]
            q2 = P.dve(lambda e: e.tensor_tensor(out=dl, in0=top8[:, 1:2], in1=top8[:, 0:1], op=ALU.subtract), [r8_])
            q3 = P.act(lambda e: e.activation(out=rq[:, 33:34], in_=dl, func=AF.Exp), [q2])
            q4_ = P.dve(lambda e: e.tensor_scalar(out=rq[:, 34:35], in0=rq[:, 33:34], scalar1=1.0, scalar2=None, op0=ALU.add), [q3])
            q5 = P.dve(lambda e: e.reciprocal(out=rq[:, 35:36], in_=rq[:, 34:35]), [q4_])
            q6 = P.dve(lambda e, t=t: e.tensor_tensor(out=wts[:, 2 * t:2 * t + 1], in0=rq[:, 35:36], in1=rs[:, 7:8], op=ALU.mult), [q5, r5_])
            q7 = P.dve(lambda e, t=t: e.tensor_tensor(out=wts[:, 2 * t + 1:2 * t + 2], in0=rs[:, 7:8], in1=wts[:, 2 * t:2 * t + 1],
                                                      op=ALU.subtract), [q6])
            ohs = rq[:, 36:68]
            q8_ = P.dve(lambda e: e.tensor_tensor(out=ohs, in0=rs[:, 96:128], in1=oh2, op=ALU.add), [r9_, q1])
            evr = P.pe(lambda e: e.matmul(bank(3, 32), lhsT=tri_f, rhs=ohs, start=True, stop=True), [q8_, ev_k6, bfree.get(3)])
            evc = P.pe(lambda e: e.matmul(bank(3, 64)[:, 32:64], lhsT=ones_f, rhs=ohs, start=True, stop=True), [ev_k6b])
            rk = rq[:, 68:100]
            q9 = P.dve(lambda e: e.tensor_tensor(out=rk, in0=bank(3, 32), in1=cntb[:, 0:32], op=ALU.add), [evr, ev_cnt])
            q10 = P.dve(lambda e: e.tensor_scalar(out=rk, in0=rk, scalar1=float(CAP - 1), scalar2=None, op0=ALU.min), [q9])
            q11 = P.dve(lambda e: e.tensor_tensor(out=rk, in0=rk, in1=ecap[:, 0:32], op=ALU.add), [q10, ev_k6d])
            ev_cnt = P.dve(lambda e: e.tensor_tensor(out=cntb[:, 0:32], in0=cntb[:, 0:32], in1=bank(3, 64)[:, 32:64], op=ALU.add),
                           [evc, q9])
            bfree[3] = ev_cnt
            sf = rq[:, 100:102]
            tm_ = rq[:, 104:136]
            q12 = P.dve(lambda e: e.tensor_tensor(out=tm_, in0=rk, in1=rs[:, 96:128], op=ALU.mult), [q11])
            q13 = P.dve(lambda e: e.reduce_sum(out=sf[:, 0:1], in_=tm_, axis=AX.X), [q12])
            q14 = P.dve(lambda e: e.tensor_tensor(out=tm_, in0=rk, in1=oh2, op=ALU.mult), [q13])
            q15 = P.dve(lambda e: e.reduce_sum(out=sf[:, 1:2], in_=tm_, axis=AX.X), [q14])
            q16 = P.dve(lambda e, t=t: e.tensor_copy(out=slots_i[:, 2 * t:2 * t + 2], in_=sf), [q15])
            ev_rs_free = q16
            sc = None
            for k in range(2):
                sc = P.op("pool", lambda e, t=t, k=k: e.indirect_dma_start(
                    out=DR["h2_scr"], out_offset=bass.IndirectOffsetOnAxis(ap=slots_i[:, 2 * t + k:2 * t + k + 1], axis=0),
                    in_=hbb, in_offset=None), [q16, a6], True, "scat", True)
            ev_hbb_free = sc
        ev_scat = ev_hbb_free
        if stop == 6:
            return finish([(dbg_d[0:128, 0:16], wts, "sp"), (dbg_d[128:256, 0:16], slots_i.bitcast(F32), "sp"),
                           (dbg_d[256:384, 0:32], cntb[:, 0:32], "sp")])
        P.barrier()
        A.release(m6)

        hblk = A.alloc(D * 2, BF16)
        hbT = A.alloc(KC * 128 * 2, BF16).rearrange("p (c n) -> p c n", c=KC)
        gsb = A.alloc(2048)
        hid = A.alloc(DE * 2, BF16)
        hidT = A.alloc(12 * 128 * 2, BF16).rearrange("p (c n) -> p c n", c=12)
        ysb = [A.alloc(2048) for _ in range(2)]
        ev_hblk_free = ev_hbT_free = ev_gsb_free = ev_hid_free = ev_hidT_free = None
        ev_ysb_free = [None, None]
        yi = 0
        ev_y_all = None
        for ex in range(NE):
            lb = P.dma("sp", hblk, DR["h2_scr"][ex * CAP:(ex + 1) * CAP, :], "hblk", [ev_hblk_free, ev_scat])
            evE = None
            for q4 in range(4):
                b = q4 % 2
                pb = bankbf(b).rearrange("p (c n) -> p c n", c=8)
                for cc in range(8):
                    c = q4 * 8 + cc
                    evt = P.pe(lambda e, pb=pb, cc=cc, c=c: e.transpose(out=pb[:, cc, :], in_=hblk[:, c * 128:(c + 1) * 128],
                                                                        identity=ident_b), [lb, bfree.get(b), ev_hbT_free],
                               sig=(cc == 7))
                gsl = gcols[:, KC + q4 * 8:KC + (q4 + 1) * 8].unsqueeze(2).to_broadcast([128, 8, 128])
                evE = P.dve(lambda e, pb=pb, q4=q4, gsl=gsl: e.tensor_tensor(out=hbT[:, q4 * 8:(q4 + 1) * 8, :], in0=pb,
                                                                             in1=gsl, op=ALU.mult), [evt])
                bfree[b] = evE
            ev_hblk_free = evt
            for cb in range(3):
                jg = w_jobs("w_gate", ex * D, D, cb * 512) if False else None
                jobs_g = []
                jobs_u = []
                for nm, lst in (("w_gate", jobs_g), ("w_up", jobs_u)):
                    for k0 in (0, 16):
                        def src(nm=nm, k0=k0, ex=ex, cb=cb):
                            return DR[nm][ex, k0 * 128:(k0 + 16) * 128, cb * 512:(cb + 1) * 512].rearrange("(c p) n -> p c n", p=128)
                        j, slot, evl = ring.get([(slot16, src)])
                        lst.append((j, slot16(slot), evl, k0, 16))
                evg = evu = None
                for c in range(KC):
                    jb = job_for(jobs_g, c)
                    lastj = (c - jb[3] == jb[4] - 1)
                    evg = P.pe(lambda e, jb=jb, c=c: e.matmul(bank(2), lhsT=hbT[:, c, :], rhs=jb[1][:, c - jb[3], :],
                                                              start=(c == 0), stop=(c == KC - 1)),
                               [jb[2], evE, bfree.get(2)], sig=(c == KC - 1 or lastj))
                    if lastj:
                        ring.done(jb[0], evg)
                for c in range(KC):
                    jb = job_for(jobs_u, c)
                    lastj = (c - jb[3] == jb[4] - 1)
                    evu = P.pe(lambda e, jb=jb, c=c: e.matmul(bank(3), lhsT=hbT[:, c, :], rhs=jb[1][:, c - jb[3], :],
                                                              start=(c == 0), stop=(c == KC - 1)),
                               [jb[2], evE, bfree.get(3)], sig=(c == KC - 1 or lastj))
                    if lastj:
                        ring.done(jb[0], evu)
                s1 = P.act(lambda e: e.activation(out=gsb[:, 0:512], in_=bank(2), func=AF.Silu), [evg, ev_gsb_free])
                bfree[2] = s1
                s2 = P.dve(lambda e, cb=cb: e.tensor_tensor(out=hid[:, cb * 512:(cb + 1) * 512], in0=bank(3), in1=gsb[:, 0:512],
                                                            op=ALU.mult), [s1, evu, ev_hid_free])
                bfree[3] = s2
                ev_gsb_free = s2
            ev_hbT_free = evu
            pbh = bankbf(4).rearrange("p (c n) -> p c n", c=8)
            pbh2 = bankbf(5).rearrange("p (c n) -> p c n", c=8)
            for c in range(12):
                pbx = pbh if c < 8 else pbh2
                evt = P.pe(lambda e, pbx=pbx, c=c: e.transpose(out=pbx[:, c % 8, :], in_=hid[:, c * 128:(c + 1) * 128],
                                                               identity=ident_b), [s2, bfree.get(4), bfree.get(5), ev_hidT_free],
                           sig=(c in (7, 11)))
                if c == 7:
                    evt8 = evt
            ev_hid_free = evt
            h1 = P.act(lambda e, pbh=pbh: e.activation(out=hidT[:, 0:8, :], in_=pbh, func=AF.Copy), [evt8])
            h2_ = P.act(lambda e, pbh2=pbh2: e.activation(out=hidT[:, 8:12, :], in_=pbh2[:, 0:4, :], func=AF.Copy), [evt])
            bfree[4] = h1
            bfree[5] = h2_
            for cb in range(8):
                def src(ex=ex, cb=cb):
                    return DR["w_down"][ex, :, cb * 512:(cb + 1) * 512].rearrange("(c p) n -> p c n", p=128)

                def dstf(slot):
                    return slot[:, 0:12 * 512].rearrange("p (c n) -> p c n", c=12)
                j, slot, evl = ring.get([(dstf, src)])
                sv = dstf(slot)
                b = 6 + cb % 2
                for c in range(12):
                    evd = P.pe(lambda e, b=b, sv=sv, c=c: e.matmul(bank(b), lhsT=hidT[:, c, :], rhs=sv[:, c, :],
                                                                   start=(c == 0), stop=(c == 11)),
                               [evl, h2_, h1, bfree.get(b)], sig=(c == 11))
                ring.done(j, evd)
                k = yi % 2
                yi += 1
                yb_ = ysb[k]
                yc = P.act(lambda e, yb_=yb_, b=b: e.activation(out=yb_[:, 0:512], in_=bank(b), func=AF.Copy),
                           [evd, ev_ysb_free[k]])
                bfree[b] = yc
                ev_ysb_free[k] = P.dma("sp", DR["y_scr"][ex * CAP:(ex + 1) * CAP, cb * 512:(cb + 1) * 512], yb_[:, 0:512],
                                       "ysb%d" % k, [yc])
            ev_hidT_free = evd
        if stop == 7:
            return finish([(dbg_d[0:4096, :], DR["y_scr"], "sp")])
        P.barrier()
        A.release(m6)

        dscr("x3_scr", [TOK, D])
        dscr("x4_scr", [TOK, D])
        h3T = hT_own
        pT = A.alloc(2 * TOK * 2, BF16).rearrange("p (c n) -> p c n", c=2)
        m8 = A.mark()
        x3t = A.alloc(D * 4)
        y0 = A.alloc(D * 4)
        hb3 = A.alloc(D * 2, BF16)
        ppt = A.alloc(1024)
        ppb = A.alloc(512, BF16)
        r8 = A.alloc(64)
        ev_x3_free = ev_y_free = ev_hb3_free = ev_pp_free = ev_ppb_free = None
        for t in range(NT):
            lx = P.dma("sp", x3t, DR["x2_scr"][t * 128:(t + 1) * 128, :], "x3t", [ev_x3_free])
            g0 = P.op("pool", lambda e, t=t: e.indirect_dma_start(
                out=y0, out_offset=None, in_=DR["y_scr"],
                in_offset=bass.IndirectOffsetOnAxis(ap=slots_i[:, 2 * t:2 * t + 1], axis=0)),
                [ev_y_free, ev_ysb_free[0], ev_ysb_free[1]], True, "gat", True)
            b1 = P.dve(lambda e, t=t: e.scalar_tensor_tensor(out=x3t, in0=y0, scalar=wts[:, 2 * t:2 * t + 1], in1=x3t,
                                                             op0=ALU.mult, op1=ALU.add), [lx, g0])
            g1 = P.op("pool", lambda e, t=t: e.indirect_dma_start(
                out=y0, out_offset=None, in_=DR["y_scr"],
                in_offset=bass.IndirectOffsetOnAxis(ap=slots_i[:, 2 * t + 1:2 * t + 2], axis=0)), [b1], True, "gat", True)
            b2 = P.dve(lambda e, t=t: e.scalar_tensor_tensor(out=x3t, in0=y0, scalar=wts[:, 2 * t + 1:2 * t + 2], in1=x3t,
                                                             op0=ALU.mult, op1=ALU.add), [g1])
            ev_y_free = b2
            st3 = P.dma("sp", DR["x3_scr"][t * 128:(t + 1) * 128, :], x3t, "x3s", [b2])
            a1 = P.act(lambda e: e.activation(out=hb3, in_=x3t, func=AF.Square, accum_out=r8[:, 0:1]), [b2, ev_hb3_free])
            a2 = P.dve(lambda e: e.tensor_scalar(out=r8[:, 1:2], in0=r8[:, 0:1], scalar1=1.0 / D, scalar2=EPS,
                                                 op0=ALU.mult, op1=ALU.add), [a1])
            a3 = P.act(lambda e: e.activation(out=r8[:, 2:3], in_=r8[:, 1:2], func=AF.Sqrt), [a2])
            a4 = P.dve(lambda e: e.reciprocal(out=r8[:, 3:4], in_=r8[:, 2:3]), [a3])
            a5 = P.act(lambda e: e.activation(out=hb3, in_=x3t, func=AF.Copy, scale=r8[:, 3:4]), [a4])
            ev_x3_free = None
            evx = [a5, st3]
            evE = None
            for q4 in range(4):
                b = q4 % 2
                pb = bankbf(b).rearrange("p (c n) -> p c n", c=8)
                for cc in range(8):
                    c = q4 * 8 + cc
                    evt = P.pe(lambda e, pb=pb, cc=cc, c=c: e.transpose(out=pb[:, cc, :], in_=hb3[:, c * 128:(c + 1) * 128],
                                                                        identity=ident_b), [a5, bfree.get(b)], sig=(cc == 7))
                gsl = gcols[:, 2 * KC + q4 * 8:2 * KC + (q4 + 1) * 8].unsqueeze(2).to_broadcast([128, 8, 128])
                evE = P.dve(lambda e, pb=pb, q4=q4, gsl=gsl, t=t: e.tensor_tensor(
                    out=h3T[:, q4 * 8:(q4 + 1) * 8, 2 + t * 128:2 + (t + 1) * 128], in0=pb, in1=gsl, op=ALU.mult), [evt])
                bfree[b] = evE
            ev_hb3_free = evt
            ev_x3_free = P.op("sp", lambda e: e.nop(), [st3, a5], True, "x3n", False)
            lp = P.dma("sp", ppt[:, 0:256], DR["pp"][t * 128:(t + 1) * 128, :], "ppt", [ev_pp_free])
            cpb = P.dve(lambda e: e.tensor_copy(out=ppb, in_=ppt[:, 0:256]), [lp, ev_ppb_free])
            ev_pp_free = cpb
            pb2 = bankbf(4)[:, 0:256].rearrange("p (c n) -> p c n", c=2)
            for c in range(2):
                evt = P.pe(lambda e, pb2=pb2, c=c: e.transpose(out=pb2[:, c, :], in_=ppb[:, c * 128:(c + 1) * 128],
                                                               identity=ident_b), [cpb, bfree.get(4)], sig=(c == 1))
            ev_ppb_free = evt
            ev_pT = P.act(lambda e, pb2=pb2, t=t: e.activation(out=pT[:, :, t * 128:(t + 1) * 128], in_=pb2, func=AF.Copy), [evt])
            bfree[4] = ev_pT
        P.barrier()
        A.release(m8)
        xs2 = [A.alloc(2048) for _ in range(2)]
        sg2 = A.alloc(2048)
        ev_xs2_free = [None, None]
        ev_sg2_free = [None]
        xj = [0]
        ev_ml = None

        def ple_epilogue(cb):
            def ep(t, bks, evs):
                k = xj[0] % 2
                xj[0] += 1
                xb = xs2[k]
                l = P.dma("sp", xb[:, 0:512], DR["x3_scr"][t * 128:(t + 1) * 128, cb * 512:(cb + 1) * 512],
                          "xq%d" % k, [ev_xs2_free[k]])
                s1 = P.act(lambda e, b=bks[0]: e.activation(out=sg2[:, 0:512], in_=bank(b), func=AF.Sigmoid),
                           [evs[0], ev_sg2_free[0]])
                bfree[bks[0]] = s1
                s2 = P.dve(lambda e, b=bks[1]: e.tensor_tensor(out=sg2[:, 0:512], in0=bank(b), in1=sg2[:, 0:512], op=ALU.mult),
                           [s1, evs[1]])
                bfree[bks[1]] = s2
                s3 = P.dve(lambda e, xb=xb: e.tensor_tensor(out=xb[:, 0:512], in0=xb[:, 0:512], in1=sg2[:, 0:512], op=ALU.add),
                           [s2, l])
                ev_sg2_free[0] = s3
                ev_xs2_free[k] = P.dma("sp", DR["x4_scr"][t * 128:(t + 1) * 128, cb * 512:(cb + 1) * 512], xb[:, 0:512],
                                       "xq%d" % k, [s3])
            return ep

        for cb in range(8):
            jobs1 = w_jobs("w_ple_gate", 0, D, cb * 512)
            jobs2 = w_jobs("w_ple_proj", 0, 256, cb * 512)
            tm_gemm([jobs1, jobs2], [KC, 2],
                    [lambda c, t: h3T[:, c, 2 + t * 128:2 + (t + 1) * 128], lambda c, t: pT[:, c, t * 128:(t + 1) * 128]],
                    [(2, 3), (4, 5)], ple_epilogue(cb))
        P.barrier()
        A.release(m_stage)
        gf = A.alloc(D * 4)
        x4t = [A.alloc(D * 4) for _ in range(2)]
        jk = A.alloc(D * 2, BF16)
        r9 = A.alloc(64)
        ev_gf = P.dma("sp", gf, DR["g_fin"][0, :].partition_broadcast(128), "c0")
        ev_x4_free = [None, None]
        last = None
        ev_jk = None
        for t in range(NT):
            k = t % 2
            xb = x4t[k]
            l = P.dma("sp", xb, DR["x4_scr"][t * 128:(t + 1) * 128, :], "x4%d" % k, [ev_x4_free[k]])
            a1 = P.act(lambda e, xb=xb, k=k: e.activation(out=jk, in_=xb, func=AF.Square, accum_out=r9[:, 4 * k:4 * k + 1]), [l, ev_jk])
            ev_jk = a1
            a2 = P.dve(lambda e, k=k: e.tensor_scalar(out=r9[:, 4 * k + 1:4 * k + 2], in0=r9[:, 4 * k:4 * k + 1], scalar1=1.0 / D,
                                                      scalar2=EPS, op0=ALU.mult, op1=ALU.add), [a1])
            a3 = P.act(lambda e, k=k: e.activation(out=r9[:, 4 * k + 2:4 * k + 3], in_=r9[:, 4 * k + 1:4 * k + 2], func=AF.Sqrt), [a2])
            a4 = P.dve(lambda e, k=k: e.reciprocal(out=r9[:, 4 * k + 3:4 * k + 4], in_=r9[:, 4 * k + 2:4 * k + 3]), [a3])
            a5 = P.dve(lambda e, xb=xb, k=k: e.scalar_tensor_tensor(out=xb, in0=xb, scalar=r9[:, 4 * k + 3:4 * k + 4], in1=gf,
                                                                    op0=ALU.mult, op1=ALU.mult), [a4, ev_gf])
            ev_x4_free[k] = P.dma("sp", out_d[t * 128:(t + 1) * 128, :], xb, "x4%d" % k, [a5])
            last = ev_x4_free[k]
        P.op("sp", lambda e: e.nop(), [ev_x4_free[0], ev_x4_free[1]], sig=False)
        P.emit(block)
    return nc, ring.rec


def build(stop=99, debug=False):
    _, specs = _build(None, stop, debug)
    nc, _ = _build(specs, stop, debug)
    return nc


def host_inputs(x, p, w_in, w_conv, w_up_a, w_out_b, w_o, norm_mix, norm_ffn, w_group, b_group,
                w_router, b_router, w_gate, w_up, w_down, norm_ple, w_ple_gate, w_ple_proj, norm_final):
    f = np.float32
    x2 = np.asarray(x, f).reshape(S, D)
    p2 = np.asarray(p, f).reshape(S, 256)
    pos = np.arange(-HALO, S, dtype=np.float64)
    half = 64
    inv = 10000.0 ** (-np.arange(half, dtype=np.float64) / half)
    ang = (np.maximum(pos, 0).astype(np.float32)[:, None] * inv.astype(np.float32)[None, :]).astype(np.float32)
    cos, sin = np.cos(ang).astype(f), np.sin(ang).astype(f)
    cos128 = np.concatenate([cos, cos], 1)
    sin128 = np.concatenate([-sin, sin], 1)
    cosF = np.tile(cos128, (1, 4))
    sinF = np.tile(sin128, (1, 4))
    xpad = np.concatenate([np.zeros((HALO, D), f), x2], 0)
    i_ = np.arange(128)[:, None]
    j_ = np.arange(256)[None, :]
    band = np.where((j_ >= i_) & (j_ <= i_ + 128), 0.0, NEG).astype(f)
    ident = np.eye(128, dtype=f)
    tri = (np.arange(128)[:, None] < np.arange(128)[None, :]).astype(f)

    def col(gv):
        return np.asarray(gv, f).reshape(KC, 128).T

    gcols = np.ascontiguousarray(np.concatenate([col(norm_mix), col(norm_ffn), col(norm_ple)], 1))
    wconv = np.ascontiguousarray(np.asarray(w_conv, f).reshape(3, 16, 128).transpose(2, 1, 0).reshape(128, 48))
    shared = {
        "band": band, "ident": ident, "tri": tri, "gcols": gcols,
        "g_fin": np.asarray(norm_final, f).reshape(1, D), "wconv": wconv,
        "w_in": np.asarray(w_in, f).reshape(D, INC), "w_up_a": np.asarray(w_up_a, f).reshape(512, D),
        "w_out_b": np.asarray(w_out_b, f).reshape(CW, D), "w_o": np.asarray(w_o, f).reshape(D, D),
        "w_rt": np.ascontiguousarray(np.concatenate([np.asarray(w_group, f).reshape(D, 4),
                                                     np.asarray(w_router, f).reshape(D, 32)], 1)),
        "b_rt": np.concatenate([np.asarray(b_group, f).reshape(1, 4), np.asarray(b_router, f).reshape(1, 32)], 1),
        "w_gate": np.asarray(w_gate, f).reshape(NE, D, DE), "w_up": np.asarray(w_up, f).reshape(NE, D, DE),
        "w_down": np.asarray(w_down, f).reshape(NE, DE, D),
        "w_ple_gate": np.asarray(w_ple_gate, f).reshape(D, D), "w_ple_proj": np.asarray(w_ple_proj, f).reshape(256, D),
    }
    maps = []
    for c in range(NCORE):
        a = c * TOK
        m = dict(shared)
        m["xh"] = xpad[a:a + TOK + HALO]
        m["pp"] = p2[a:a + TOK]
        m["cosF"] = cosF[a:a + TOK + HALO]
        m["sinF"] = sinF[a:a + TOK + HALO]
        m["km"] = np.where(pos[a:a + TOK + HALO] >= 0, 0.0, NEG).astype(f).reshape(1, -1)
        maps.append(m)
    return maps


def kernel(**inputs):
    maps = host_inputs(**inputs)
    nc = build()
    res = run_bass_kernel_spmd(nc, maps, core_ids=list(range(NCORE)))
    out = np.concatenate([r["out"] for r in res.results], axis=0)
    return out.reshape(1, S, D).astype(np.float32)
```
